# Optimizing a Trainium2 kernel written in Bass

```python
import jax
import jax.numpy as jnp
from jax import lax
import numpy as np

D_MODEL = 4096
BATCH = 1
SEQ = 8192
DEPTH = 4

GRID_W = 64
CTX_LEN = 256
HEAD_DIM = 128
BRANCH_W = 1024
N_BRANCHES = 3
CHUNK = 128
GMLP_GROUPS = 8
GMLP_GROUP_DIM = BRANCH_W // GMLP_GROUPS
NA_HEADS = BRANCH_W // HEAD_DIM
NA_WIN_R = 8
NA_WIN_C = 16
SWA_HEADS = BRANCH_W // HEAD_DIM
SWA_KV_HEADS = 2
SWA_KV_W = SWA_KV_HEADS * HEAD_DIM
SWA_WINDOW = 128
SWA_BLOCK = 128
ROPE_BASE = 10000.0
FFN_DENSE = 2048
N_EXPERTS = 8
TOP_K = 2
FFN_EXPERT = 384
N_DENSE_LAYERS = (DEPTH + 1) // 2
N_MOE_LAYERS = DEPTH // 2

OFF_AU = 0
OFF_AV = OFF_AU + BRANCH_W
OFF_BQ = OFF_AV + BRANCH_W
OFF_CQ = OFF_BQ + BRANCH_W
OFF_GATE = OFF_CQ + BRANCH_W
OFF_KV = OFF_GATE + N_BRANCHES * D_MODEL
OFF_BK = OFF_KV
OFF_BV = OFF_BK + BRANCH_W
OFF_CK = OFF_BV + BRANCH_W
OFF_CV = OFF_CK + SWA_KV_W
P_TOTAL = OFF_CV + SWA_KV_W
NEG_INF = -1e30

kernel_name = "hybrid_gmlp_natten_swa_moe_dit"


def rmsnorm(x, g, eps=1e-6):
    xf = x.astype(jnp.float32)
    y = xf * lax.rsqrt(jnp.mean(xf * xf, axis=-1, keepdims=True) + eps)
    return y.astype(x.dtype) * g


def layernorm(x, g, b, eps=1e-6):
    xf = x.astype(jnp.float32)
    mu = jnp.mean(xf, axis=-1, keepdims=True)
    var = jnp.mean(jnp.square(xf - mu), axis=-1, keepdims=True)
    return ((xf - mu) * lax.rsqrt(var + eps)).astype(x.dtype) * g + b


def modulate(h, shift, scale):
    return h * (1.0 + scale) + shift


def _heads(t):
    return t.reshape(t.shape[0], t.shape[1], -1, HEAD_DIM)


def _rope_1d(x, pos):
    m = x.shape[-1]
    inv = ROPE_BASE ** (-jnp.arange(0, m, 2, dtype=jnp.float32) / m)
    ang = pos.astype(jnp.float32)[:, None] * inv[None, :]
    cos, sin = jnp.cos(ang)[:, None, :], jnp.sin(ang)[:, None, :]
    xf = x.astype(jnp.float32)
    x1, x2 = xf[..., : m // 2], xf[..., m // 2:]
    return jnp.concatenate([x1 * cos - x2 * sin, x2 * cos + x1 * sin], axis=-1).astype(x.dtype)


def axial_rope(x, pos_r, pos_c):
    h = x.shape[-1] // 2
    return jnp.concatenate([_rope_1d(x[..., :h], pos_r), _rope_1d(x[..., h:], pos_c)], axis=-1)


def chunk_gmlp(u, v, ln_g, ln_b, w_s, b_s):
    bn, n, w = u.shape
    u = jax.nn.gelu(u)
    v = layernorm(jax.nn.gelu(v), ln_g, ln_b)
    v = v.reshape(bn, n // CHUNK, CHUNK, GMLP_GROUPS, GMLP_GROUP_DIM)
    s = jnp.einsum('gpq,bnqgc->bnpgc', w_s, v) + b_s.T[:, :, None]
    return u * s.reshape(bn, n, w)


def neighbourhood_attention(q, k, v, k_ctx, v_ctx, rpb):
    bn, n, h, d = q.shape
    rows = n // GRID_W
    kr = min(NA_WIN_R, rows)
    r = jnp.arange(rows)
    key_rows = jnp.clip(r - kr // 2, 0, rows - kr)[:, None] + jnp.arange(kr)[None, :]
    col = jnp.arange(GRID_W)
    c0 = jnp.clip(col - NA_WIN_C // 2, 0, GRID_W - NA_WIN_C)
    in_win = (col[None, :] >= c0[:, None]) & (col[None, :] < c0[:, None] + NA_WIN_C)
    qg = q.reshape(bn, rows, GRID_W, h, d)
    kg = k.reshape(bn, rows, GRID_W, h, d)[:, key_rows]
    vg = v.reshape(bn, rows, GRID_W, h, d)[:, key_rows]
    scale = d ** -0.5
    s_loc = jnp.einsum('brqhd,brjkhd->brhqjk', qg, kg, preferred_element_type=jnp.float32) * scale
    roff = key_rows - r[:, None] + (NA_WIN_R - 1)
    coff = jnp.clip(col[None, :] - col[:, None] + (NA_WIN_C - 1), 0, 2 * NA_WIN_C - 2)
    bias = rpb[:, roff[:, :, None, None], coff[None, None]].transpose(1, 0, 3, 2, 4)
    s_loc = jnp.where(in_win[:, None, :], s_loc + bias[None].astype(jnp.float32), NEG_INF)
    s_loc = s_loc.reshape(bn, rows, h, GRID_W, kr * GRID_W)
    s_ctx = jnp.einsum('brqhd,bchd->brhqc', qg, k_ctx, preferred_element_type=jnp.float32) * scale
    p = jax.nn.softmax(jnp.concatenate([s_loc, s_ctx], axis=-1), axis=-1)
    p_loc = p[..., : kr * GRID_W].reshape(bn, rows, h, GRID_W, kr, GRID_W).astype(v.dtype)
    p_ctx = p[..., kr * GRID_W:].astype(v.dtype)
    o = jnp.einsum('brhqjk,brjkhd->brqhd', p_loc, vg) + jnp.einsum('brhqc,bchd->brqhd', p_ctx, v_ctx)
    return o.reshape(bn, n, h * d)


def window_gqa(q, k, v, k_ctx, v_ctx, sink):
    bn, n, hq, d = q.shape
    hk = k.shape[2]
    g = hq // hk
    blk = SWA_BLOCK
    nb = n // blk
    pad = ((0, 0), (blk, blk), (0, 0), (0, 0))
    kp = jnp.pad(k, pad).reshape(bn, nb + 2, blk, hk, d)
    vp = jnp.pad(v, pad).reshape(bn, nb + 2, blk, hk, d)
    kw = jnp.concatenate([kp[:, :-2], kp[:, 1:-1], kp[:, 2:]], axis=2)
    vw = jnp.concatenate([vp[:, :-2], vp[:, 1:-1], vp[:, 2:]], axis=2)
    qb = q.reshape(bn, nb, blk, hk, g, d)
    scale = d ** -0.5
    s = jnp.einsum('bnqkgd,bnskd->bnkgqs', qb, kw, preferred_element_type=jnp.float32) * scale
    a = jnp.arange(blk)
    j = jnp.arange(3 * blk)
    rel = (j[None, :] - blk) - a[:, None]
    kpos = jnp.arange(nb)[:, None] * blk - blk + j[None, :]
    valid = (jnp.abs(rel) <= SWA_WINDOW)[None] & ((kpos >= 0) & (kpos < n))[:, None, :]
    s = jnp.where(valid[:, None, None], s, NEG_INF)
    s_ctx = jnp.einsum('bnqkgd,bckd->bnkgqc', qb, k_ctx, preferred_element_type=jnp.float32) * scale
    sink_col = jnp.broadcast_to(sink.reshape(hk, g)[:, :, None, None].astype(jnp.float32), s.shape[:-1] + (1,))
    p = jax.nn.softmax(jnp.concatenate([s, s_ctx, sink_col], axis=-1), axis=-1)
    n_ctx = k_ctx.shape[1]
    p_loc = p[..., : 3 * blk].astype(v.dtype)
    p_ctx = p[..., 3 * blk: 3 * blk + n_ctx].astype(v.dtype)
    o = jnp.einsum('bnkgqs,bnskd->bnqkgd', p_loc, vw) + jnp.einsum('bnkgqc,bckd->bnqkgd', p_ctx, v_ctx)
    return o.reshape(bn, n, hq * d)


def context_attention(q, k, v, sink):
    bn, n, hq, d = q.shape
    hk = k.shape[2]
    g = hq // hk
    qg = q.reshape(bn, n, hk, g, d)
    s = jnp.einsum('bqkgd,bskd->bkgqs', qg, k, preferred_element_type=jnp.float32) * (d ** -0.5)
    n_keys = k.shape[1]
    if sink is not None:
        sink_col = jnp.broadcast_to(sink.reshape(hk, g)[:, :, None, None].astype(jnp.float32), s.shape[:-1] + (1,))
        s = jnp.concatenate([s, sink_col], axis=-1)
    p = jax.nn.softmax(s, axis=-1)[..., :n_keys].astype(v.dtype)
    o = jnp.einsum('bkgqs,bskd->bqkgd', p, v)
    return o.reshape(bn, n, hq * d)


def merge_branches(ys, p_gate, w_branch, w_out):
    gates = jax.nn.sigmoid(p_gate.astype(jnp.float32)).astype(p_gate.dtype)
    acc = gates[..., :D_MODEL] * (ys[0] @ w_branch[0])
    for i in range(1, N_BRANCHES):
        acc = acc + gates[..., i * D_MODEL:(i + 1) * D_MODEL] * (ys[i] @ w_branch[i])
    return acc @ w_out


def swiglu(h, w_gate, w_up, w_down):
    return (jax.nn.silu(h @ w_gate) * (h @ w_up)) @ w_down


def moe_swiglu(h, w_router, w_gate, w_up, w_down):
    logits = jnp.einsum('btd,de->bte', h, w_router, preferred_element_type=jnp.float32)
    top_v, top_i = lax.top_k(logits, TOP_K)
    wts = jax.nn.softmax(top_v, axis=-1)
    gate = jnp.sum(jax.nn.one_hot(top_i, N_EXPERTS, dtype=jnp.float32) * wts[..., None], axis=-2)
    hid = jax.nn.silu(jnp.einsum('btd,edf->btef', h, w_gate)) * jnp.einsum('btd,edf->btef', h, w_up)
    hid = hid * gate.astype(h.dtype)[..., None]
    return jnp.einsum('btef,efd->btd', hid, w_down)


def channel_mixer(h, l, w_ffn_gate, w_ffn_up, w_ffn_down, w_router, w_exp_gate, w_exp_up, w_exp_down):
    i = l // 2
    if l % 2 == 0:
        return swiglu(h, w_ffn_gate[i], w_ffn_up[i], w_ffn_down[i])
    return moe_swiglu(h, w_router[i], w_exp_gate[i], w_exp_up[i], w_exp_down[i])


def setup_inputs(seed: int = 0) -> dict:
    key = jax.random.key(seed)
    ks = jax.random.split(key, 32)
    f32 = jnp.float32
    L, D = DEPTH, D_MODEL

    def nrm(k, shape, scale):
        return jax.random.normal(k, shape, f32) * scale

    return {
        "x": nrm(ks[0], (BATCH, SEQ, D), 1.0),
        "c": nrm(ks[1], (BATCH, D), 1.0),
        "ctx": nrm(ks[2], (BATCH, CTX_LEN, D), 1.0),
        "c_ctx": nrm(ks[3], (D,), 1.0),
        "w_ada": nrm(ks[4], (L, D, 6 * D), 0.5 * D ** -0.5),
        "b_ada": nrm(ks[5], (L, 6 * D), 0.02),
        "g_mix": 1.0 + nrm(ks[6], (L, D), 0.05),
        "w_in": nrm(ks[7], (L, D, P_TOTAL), D ** -0.5),
        "gmlp_ln_g": 1.0 + nrm(ks[8], (L, BRANCH_W), 0.05),
        "gmlp_ln_b": nrm(ks[9], (L, BRANCH_W), 0.02),
        "gmlp_ws": nrm(ks[10], (L, GMLP_GROUPS, CHUNK, CHUNK), CHUNK ** -0.5),
        "gmlp_bs": 1.0 + nrm(ks[11], (L, GMLP_GROUPS, CHUNK), 0.05),
        "na_rpb": nrm(ks[12], (L, NA_HEADS, 2 * NA_WIN_R - 1, 2 * NA_WIN_C - 1), 0.1),
        "swa_sink": nrm(ks[13], (L, SWA_HEADS), 1.0),
        "w_branch": nrm(ks[14], (L, N_BRANCHES, BRANCH_W, D), BRANCH_W ** -0.5),
        "w_out": nrm(ks[15], (L, D, D), D ** -0.5),
        "g_ffn": 1.0 + nrm(ks[16], (L, D), 0.05),
        "w_ffn_gate": nrm(ks[17], (N_DENSE_LAYERS, D, FFN_DENSE), D ** -0.5),
        "w_ffn_up": nrm(ks[18], (N_DENSE_LAYERS, D, FFN_DENSE), D ** -0.5),
        "w_ffn_down": nrm(ks[19], (N_DENSE_LAYERS, FFN_DENSE, D), FFN_DENSE ** -0.5),
        "w_router": nrm(ks[20], (N_MOE_LAYERS, D, N_EXPERTS), D ** -0.5),
        "w_exp_gate": nrm(ks[21], (N_MOE_LAYERS, N_EXPERTS, D, FFN_EXPERT), D ** -0.5),
        "w_exp_up": nrm(ks[22], (N_MOE_LAYERS, N_EXPERTS, D, FFN_EXPERT), D ** -0.5),
        "w_exp_down": nrm(ks[23], (N_MOE_LAYERS, N_EXPERTS, FFN_EXPERT, D), FFN_EXPERT ** -0.5),
        "g_final": 1.0 + nrm(ks[24], (D,), 0.05),
    }


def reference(x, c, ctx, c_ctx, w_ada, b_ada, g_mix, w_in, gmlp_ln_g, gmlp_ln_b, gmlp_ws, gmlp_bs, na_rpb,
              swa_sink, w_branch, w_out, g_ffn, w_ffn_gate, w_ffn_up, w_ffn_down, w_router, w_exp_gate,
              w_exp_up, w_exp_down, g_final):
    n_lat = x.shape[1]
    t = jnp.arange(n_lat, dtype=jnp.int32)
    pos_r, pos_c = t // GRID_W, t % GRID_W
    cs = jax.nn.silu(c)
    ccs = jax.nn.silu(c_ctx)
    for l in range(DEPTH):
        last = l == DEPTH - 1
        mod_x = jnp.split((cs @ w_ada[l] + b_ada[l])[:, None, :], 6, axis=-1)
        mod_c = jnp.split((ccs @ w_ada[l] + b_ada[l])[None, None, :], 6, axis=-1)

        hx = modulate(rmsnorm(x, g_mix[l]), mod_x[0], mod_x[1])
        hc = modulate(rmsnorm(ctx, g_mix[l]), mod_c[0], mod_c[1])
        px = hx @ w_in[l]
        pc = hc @ (w_in[l][:, OFF_KV:] if last else w_in[l])
        pc_kv = pc if last else pc[..., OFF_KV:]
        kb_ctx = _heads(pc_kv[..., :BRANCH_W])
        vb_ctx = _heads(pc_kv[..., BRANCH_W:2 * BRANCH_W])
        kc_ctx = _heads(pc_kv[..., 2 * BRANCH_W:2 * BRANCH_W + SWA_KV_W])
        vc_ctx = _heads(pc_kv[..., 2 * BRANCH_W + SWA_KV_W:])

        y_a = chunk_gmlp(px[..., OFF_AU:OFF_AV], px[..., OFF_AV:OFF_BQ], gmlp_ln_g[l], gmlp_ln_b[l],
                         gmlp_ws[l], gmlp_bs[l])
        y_b = neighbourhood_attention(_heads(px[..., OFF_BQ:OFF_CQ]), _heads(px[..., OFF_BK:OFF_BV]),
                                      _heads(px[..., OFF_BV:OFF_CK]), kb_ctx, vb_ctx, na_rpb[l])
        q_c = axial_rope(_heads(px[..., OFF_CQ:OFF_GATE]), pos_r, pos_c)
        k_c = axial_rope(_heads(px[..., OFF_CK:OFF_CV]), pos_r, pos_c)
        y_c = window_gqa(q_c, k_c, _heads(px[..., OFF_CV:P_TOTAL]), kc_ctx, vc_ctx, swa_sink[l])
        mix_x = merge_branches((y_a, y_b, y_c), px[..., OFF_GATE:OFF_KV], w_branch[l], w_out[l])
        x = x + mod_x[2] * mix_x

        if not last:
            yc_a = chunk_gmlp(pc[..., OFF_AU:OFF_AV], pc[..., OFF_AV:OFF_BQ], gmlp_ln_g[l], gmlp_ln_b[l],
                              gmlp_ws[l], gmlp_bs[l])
            yc_b = context_attention(_heads(pc[..., OFF_BQ:OFF_CQ]), kb_ctx, vb_ctx, None)
            yc_c = context_attention(_heads(pc[..., OFF_CQ:OFF_GATE]), kc_ctx, vc_ctx, swa_sink[l])
            mix_c = merge_branches((yc_a, yc_b, yc_c), pc[..., OFF_GATE:OFF_KV], w_branch[l], w_out[l])
            ctx = ctx + mod_c[2] * mix_c

        hx = modulate(rmsnorm(x, g_ffn[l]), mod_x[3], mod_x[4])
        x = x + mod_x[5] * channel_mixer(hx, l, w_ffn_gate, w_ffn_up, w_ffn_down, w_router, w_exp_gate,
                                          w_exp_up, w_exp_down)
        if not last:
            hc = modulate(rmsnorm(ctx, g_ffn[l]), mod_c[3], mod_c[4])
            ctx = ctx + mod_c[5] * channel_mixer(hc, l, w_ffn_gate, w_ffn_up, w_ffn_down, w_router,
                                                  w_exp_gate, w_exp_up, w_exp_down)
    return rmsnorm(x, g_final)
```

```python
from contextlib import ExitStack
import numpy as np
import concourse.bass as bass
import concourse.mybir as mybir
from concourse.bass_utils import run_bass_kernel_spmd

F32, BF16 = mybir.dt.float32, mybir.dt.bfloat16
AF = mybir.ActivationFunctionType
ALU = mybir.AluOpType
AX = mybir.AxisListType

NCORE = 8
D = 4096
KC = 32
L = 4
TL, TCX, T = 8, 2, 10
NT = T * 128
NLAT = 1024
PTOT = 18944
OFF_AU, OFF_AV, OFF_BQ, OFF_CQ, OFF_GATE, OFF_KV = 0, 1024, 2048, 3072, 4096, 16384
OFF_BK, OFF_BV, OFF_CK, OFF_CV = 16384, 17408, 18432, 18688
FFN = 2048
NE, FE = 8, 384
EPS = 1e-6
NEG = -1e30
SCALE = 128 ** -0.5
E_CTX, E_PREV, E_OWN, E_NEXT, E_TOT = 0, 256, 512, 1536, 1792
NA_START = [-4, -2, 0, 2, 4, 6, 8, 8]
NA_ROWS = [12, 10, 9, 9, 9, 9, 9, 11]
NA_W = [r * 64 for r in NA_ROWS]
NA_WMAX = 768
NA_CLS = (0, 1, 2, 6, 7)
DEBUG = False
NLAYERS = L


class Buf:
    __slots__ = ("name", "w", "r", "dsem", "dcnt", "dkey")

    def __init__(self, name):
        self.name, self.w, self.r, self.dsem, self.dcnt, self.dkey = name, None, {}, None, 0, None


class KB:
    def __init__(self, nc):
        self.nc = nc
        self.E = {"pe": nc.tensor, "act": nc.scalar, "dve": nc.vector, "pool": nc.gpsimd, "sp": nc.sync}
        self.sem = {k: nc.semaphore("es_" + k).__enter__() for k in self.E}
        self.cnt = {k: 0 for k in self.E}
        self.seen = {k: {} for k in self.E}
        self.nsem = 0
        self.grave = {}
        self.sem_pool = []

    def buf(self, name):
        b = Buf(name)
        b.r = dict(self.grave)
        return b

    def retire(self, bufs):
        for b in bufs:
            toks = list(b.r.values()) + ([b.w] if b.w is not None else [])
            for t in toks:
                o = self.grave.get(t[0])
                if o is None or o[2] < t[2]:
                    self.grave[t[0]] = t
            if b.dsem is not None:
                self.sem_pool.append((b.dsem, b.dcnt, b.dkey))
                b.dsem = None

    def _wait(self, e, tok):
        key, sem, val = tok
        if key == "pe" and e == "pe":
            return
        if self.seen[e].get(key, 0) >= val:
            return
        self.E[e].wait_ge(sem, val)
        self.seen[e][key] = val

    def _deps(self, e, reads, writes):
        for b in reads:
            if b.w is not None:
                self._wait(e, b.w)
        for b in writes:
            if b.w is not None:
                self._wait(e, b.w)
            for t in b.r.values():
                self._wait(e, t)

    def _mark(self, tok, reads, writes):
        for b in reads:
            b.r[tok[0]] = tok
        for b in writes:
            b.w = tok
            b.r = {}

    def op(self, e, fn, reads=(), writes=()):
        self._deps(e, reads, writes)
        ins = fn(self.E[e])
        self.cnt[e] += 1
        ins.then_inc(self.sem[e], 1)
        tok = (e, self.sem[e], self.cnt[e])
        self._mark(tok, reads, writes)
        return tok

    def _dsem(self, b):
        if b.dsem is None:
            if self.sem_pool:
                b.dsem, b.dcnt, b.dkey = self.sem_pool.pop(0)
            else:
                b.dsem = self.nc.semaphore("ds%d" % self.nsem).__enter__()
                b.dcnt, b.dkey = 0, ("d", self.nsem)
                self.nsem += 1
        return b.dsem

    def dma(self, q, pairs, reads=(), writes=()):
        self._deps(q, reads, writes)
        b0 = writes[0]
        s = self._dsem(b0)
        for (o, i) in pairs:
            self.E[q].dma_start(out=o, in_=i).then_inc(s, 16)
            b0.dcnt += 16
        tok = (b0.dkey, s, b0.dcnt)
        self._mark(tok, reads, writes)
        return tok

    def allgather(self, src, dst, reads, writes):
        self._deps("pool", reads, writes)
        b0 = writes[0]
        s = self._dsem(b0)
        self.nc.gpsimd.collective_compute(
            "AllGather", ALU.bypass, replica_groups=[list(range(NCORE))],
            ins=[src.ap().opt()], outs=[dst.ap().opt()]).then_inc(s)
        b0.dcnt += 1
        tok = (b0.dkey, s, b0.dcnt)
        self._mark(tok, reads, writes)
        return tok

    def drain(self, e, bufs):
        for b in bufs:
            if b.w is not None:
                self._wait(e, b.w)
            for t in b.r.values():
                self._wait(e, t)


class Phase:
    def __init__(self, kb):
        self.kb, self.es, self.bufs = kb, ExitStack(), []

    def __enter__(self):
        self.es.__enter__()
        return self

    def sb(self, name, shape, dt):
        self.kb.nsb = getattr(self.kb, "nsb", 0) + 1
        name = "%s_%d" % (name, self.kb.nsb)
        t = self.es.enter_context(self.kb.nc.sbuf_tensor(name, list(shape), dt))
        b = self.kb.buf(name)
        self.bufs.append(b)
        return t, b

    def __exit__(self, *a):
        self.kb.retire(self.bufs)
        return self.es.__exit__(*a)


def build_program():
    nc = bass.Bass("TRN2", target_bir_lowering=False)
    kb = KB(nc)
    uid = [0]

    def din(name, shape, dt=F32):
        return nc.dram_tensor(name, list(shape), dt, kind="ExternalInput")

    def dscr(name, shape, dt=F32):
        return nc.dram_tensor(name, list(shape), dt)

    x_in = din("x", [NLAT, D])
    ctx_in = din("ctx", [256, D])
    cvec_in = din("cvec", [128, KC, 2])
    wada_in = din("w_ada", [NLAYERS, D, 3072])
    bada_in = din("b_ada", [128, NCORE, L, 24])
    gmix_in = din("g_mix", [128, L, KC])
    gffn_in = din("g_ffn", [128, L, KC])
    lng_in = din("ln_g", [L, 128, 1024])
    lnb_in = din("ln_b", [L, 128, 1024])
    wst_in = din("ws_t", [L, 128, 8, 128])
    bst_in = din("bs_t", [L, 128, 8])
    sink_in = din("sink", [L, 128, 8])
    gfin_in = din("g_final", [128, D])
    nab_in = din("na_bias", [L, 8, 128, sum(NA_W[i] for i in NA_CLS)])
    swam_in = din("swa_mask", [3, 128, 384])
    rope_in = din("rope", [1280, 2, 1024])
    sel_in = din("sel", [128, 2, NCORE, 128])
    wr_in = din("w_router", [2, 128, KC, NE])
    WIN_SLABS = [(0, 4096), (4096, 8192), (8192, 12288), (12288, 16384), (16384, PTOT)]
    sh_win = [din("w_in%d" % i, [NLAYERS, D // NCORE, ce - cs]) for i, (cs, ce) in enumerate(WIN_SLABS)]
    sh_wbr = din("w_branch", [NLAYERS, 3072 // NCORE, D])
    sh_wout = din("w_out", [NLAYERS, D // NCORE, D])
    sh_fg = din("w_ffn_gate", [2, D // NCORE, FFN])
    sh_fu = din("w_ffn_up", [2, D // NCORE, FFN])
    sh_fd = din("w_ffn_down", [2, FFN // NCORE, D])
    sh_eg = din("w_exp_gate", [2, D, FE])
    sh_eu = din("w_exp_up", [2, D, FE])
    sh_ed = din("w_exp_down", [2, FE, D])
    y_out = nc.dram_tensor("y", [NLAT, D], F32, kind="ExternalOutput")

    X = dscr("X", [NT, D])
    Xb = [kb.buf("X%d" % t) for t in range(T)]
    PXA = dscr("PXA", [NT, 3072])
    PXAb = [kb.buf("PXA")] * T
    EKV = dscr("EKV", [E_TOT, 1536], BF16)
    EKVb = kb.buf("EKV")
    EKT = dscr("EKT", [1024, E_TOT], BF16)
    EKTb = kb.buf("EKT")
    QBT = dscr("QBT", [1024, NT], BF16)
    QBTb = kb.buf("QBT")
    PKT = dscr("PKT", [512, 1536], BF16)
    PKF = dscr("PKF", [1024, 512], BF16)
    GT = dscr("GT", [NCORE * 512, 1536], BF16)
    GF = dscr("GF", [NCORE * 1024, 512], BF16)
    PKTb, PKFb, GTb, GFb = kb.buf("PKT"), kb.buf("PKF"), kb.buf("GT"), kb.buf("GF")
    YT = dscr("YT", [3072, NT], BF16)
    YTb = kb.buf("YT")
    ACCT = dscr("ACCT", [D, NT], BF16)
    ACCTb = kb.buf("ACCT")
    MODP = dscr("MODP", [128, L * 24 * 2])
    MODG = dscr("MODG", [NCORE * 128, L * 24 * 2])
    MODPb, MODGb = kb.buf("MODP"), kb.buf("MODG")
    GBC = dscr("GBC", [L, 2, 2, 128, D])
    GBCb = kb.buf("GBC")

    dbgb = []

    def dbg_out(name, src, shape, dt, readbufs):
        if not DEBUG:
            return
        o = nc.dram_tensor("dbg_" + name, list(shape), dt, kind="ExternalOutput")
        b = kb.buf("dbg_" + name)
        r = shape[0]
        n = 4 if r % 4 == 0 else 1
        kb.dma("sp", [(o[i * (r // n):(i + 1) * (r // n)], src[i * (r // n):(i + 1) * (r // n)]) for i in range(n)], readbufs, [b])
        dbgb.append(b)

    def gw(name, rows, cols):
        return dscr(name, [rows, cols]), kb.buf(name)

    W_in = [[gw("Win%d_%d" % (l, i), D, ce - cs) for i, (cs, ce) in enumerate(WIN_SLABS)] for l in range(L)]
    W_br = [gw("Wbr%d" % l, 3072, D) for l in range(L)]
    W_out = [gw("Wout%d" % l, D, D) for l in range(L)]
    W_fg = [gw("Wfg%d" % i, D, FFN) for i in range(2)]
    W_fu = [gw("Wfu%d" % i, D, FFN) for i in range(2)]
    W_fd = [gw("Wfd%d" % i, FFN, D) for i in range(2)]
    W_eg = [gw("Weg%d" % i, NE * D, FE) for i in range(2)]
    W_eu = [gw("Weu%d" % i, NE * D, FE) for i in range(2)]
    W_ed = [gw("Wed%d" % i, NE * FE, D) for i in range(2)]

    def gather(shard_ap, rows, cols, dstpair, name):
        bn = dscr("bn_" + name, [rows, cols])
        bb = kb.buf("bn_" + name)
        nsp = 4 if rows % 4 == 0 else 1
        rs = rows // nsp
        kb.dma("sp", [(bn[i * rs:(i + 1) * rs, :], shard_ap[i * rs:(i + 1) * rs, :]) for i in range(nsp)], [], [bb])
        kb.allgather(bn, dstpair[0], [bb], [dstpair[1]])
        kb.retire([bb])

    def gather_layer(l):
        i = l // 2
        for si, (cs, ce) in enumerate(WIN_SLABS):
            gather(sh_win[si][l], D // NCORE, ce - cs, W_in[l][si], "win%d_%d" % (l, si))
        gather(sh_wbr[l], 384, D, W_br[l], "wbr%d" % l)
        gather(sh_wout[l], D // NCORE, D, W_out[l], "wout%d" % l)
        if l % 2 == 0:
            gather(sh_fg[i], 512, FFN, W_fg[i], "wfg%d" % i)
            gather(sh_fu[i], 512, FFN, W_fu[i], "wfu%d" % i)
            gather(sh_fd[i], 256, D, W_fd[i], "wfd%d" % i)
        else:
            gather(sh_eg[i], D, FE, W_eg[i], "weg%d" % i)
            gather(sh_eu[i], D, FE, W_eu[i], "weu%d" % i)
            gather(sh_ed[i], FE, D, W_ed[i], "wed%d" % i)

    top = ExitStack()

    def psb(name, shape, dt):
        return top.enter_context(nc.sbuf_tensor(name, list(shape), dt)), kb.buf(name)

    A, Ab = psb("A", [128, KC, NT], BF16)
    ident, identb = psb("ident", [128, 128], F32)
    identh, identhb = psb("identh", [128, 128], BF16)
    ones, onesb = psb("ones", [128, 128], F32)
    SHT, SHTb = psb("SHT", [128, L, 2, 2, KC], F32)
    AM, AMb = psb("AM", [128, L, 2, 2, KC], F32)
    ps = [top.enter_context(nc.psum_tensor("ps%d" % i, [128, 512], F32)) for i in range(4)]
    psb_ = [kb.buf("ps%d" % i) for i in range(4)]
    psS = top.enter_context(nc.psum_tensor("psS", [128, 1024], F32))
    psSb = kb.buf("psS")
    pt = [top.enter_context(nc.psum_tensor("pt%d" % i, [128, 1024], BF16)) for i in range(2)]
    ptb = [kb.buf("pt%d" % i) for i in range(2)]
    rr = {"ps": 0, "pt": 0, "ev": 0}

    def next_ps():
        i = rr["ps"] % 4
        rr["ps"] += 1
        return ps[i], psb_[i]

    def next_pt():
        i = rr["pt"] % 2
        rr["pt"] += 1
        return pt[i], ptb[i]

    def ev_eng():
        rr["ev"] += 1
        return "act" if rr["ev"] % 2 else "dve"

    def copy(e, out, in_, reads, writes):
        if e == "act":
            return kb.op("act", lambda g: g.activation(out, in_, AF.Copy), reads, writes)
        return kb.op(e, lambda g: g.tensor_copy(out, in_), reads, writes)

    kb.dma("sp", [(ident[:], din("ident_in", [128, 128])[:, :])], [], [identb])
    kb.op("dve", lambda g: g.tensor_copy(identh[:], ident[:]), [identb], [identhb])
    kb.op("dve", lambda g: g.memset(ones[:], 1.0), [], [onesb])
    for t in range(TL):
        kb.dma("sp", [(X[t * 128:(t + 1) * 128, :], x_in[t * 128:(t + 1) * 128, :])], [], [Xb[t]])
    for t in range(TCX):
        kb.dma("sp", [(X[NLAT + t * 128:NLAT + (t + 1) * 128, :], ctx_in[t * 128:(t + 1) * 128, :])], [], [Xb[TL + t]])
    gather_layer(0)

    with Phase(kb) as ph:
        cv, cvb = ph.sb("cv", [128, KC, 2], F32)
        sg, sgb = ph.sb("sg", [128, KC, 2], F32)
        modp, modpb = ph.sb("modp", [128, L, 24, 2], F32)
        MODS, MODSb = ph.sb("MODS", [128, L, 2, 6, KC], F32)
        kb.op("dve", lambda g: g.memset(modp[:], 0.0), [], [modpb])
        wa = [ph.sb("wa%d" % i, [128, KC, 128], F32) for i in range(2)]
        kb.dma("sp", [(cv[:], cvec_in[:, :, :])], [], [cvb])
        kb.op("act", lambda g: g.activation(sg[:], cv[:], AF.Sigmoid), [cvb], [sgb])
        kb.op("dve", lambda g: g.tensor_mul(cv[:], cv[:], sg[:]), [sgb, cvb], [cvb])
        n = 0
        for l in range(NLAYERS):
            for blk in range(24):
                wt, wb = wa[n % 2]
                n += 1
                kb.dma("sp", [(wt[:, k0:k0 + 8, :],
                               wada_in[l, k0 * 128:(k0 + 8) * 128, blk * 128:(blk + 1) * 128].rearrange("(k p) n -> p k n", p=128))
                              for k0 in range(0, KC, 8)], [], [wb])
                pst, pbb = next_ps()
                for k in range(KC):
                    kb.op("pe", lambda g, k=k: g.matmul(pst[:, 0:2], wt[:, k, :], cv[:, k, :], start=(k == 0), stop=(k == KC - 1)),
                          [wb, cvb], [pbb])
                kb.op("dve", lambda g: g.tensor_copy(modp[:, l, blk, :], pst[:, 0:2]), [pbb], [modpb])
        kb.dma("sp", [(MODP[:, :], modp[:].rearrange("p l b v -> p (l b v)"))], [modpb], [MODPb])
        kb.allgather(MODP, MODG, [MODPb], [MODGb])
        modt, modtb = ph.sb("modt", [128, NCORE, L, 24, 2], F32)
        bada, badab = ph.sb("bada", [128, NCORE, L, 24], F32)
        gmx, gmxb = ph.sb("gmx", [128, L, KC], F32)
        gfx, gfxb = ph.sb("gfx", [128, L, KC], F32)
        kb.dma("sp", [(modt[:].rearrange("p c l b v -> p c (l b v)"), MODG.ap().rearrange("(c p) f -> p c f", p=128))], [MODGb], [modtb])
        kb.dma("sp", [(bada[:], bada_in[:, :, :, :])], [], [badab])
        kb.dma("sp", [(gmx[:], gmix_in[:, :, :])], [], [gmxb])
        kb.dma("sp", [(gfx[:], gffn_in[:, :, :])], [], [gfxb])
        for l in range(NLAYERS):
            for v in range(2):
                for m in range(6):
                    k = 0
                    while k < KC:
                        B = m * KC + k
                        c, b = B // 24, B % 24
                        n_ = min(KC - k, 24 - b)
                        kb.op("dve", lambda g, k=k, c=c, b=b, n_=n_: g.tensor_tensor(
                            MODS[:, l, v, m, k:k + n_], modt[:, c, l, b:b + n_, v], bada[:, c, l, b:b + n_], ALU.add),
                            [modtb, badab], [MODSb])
                        k += n_
                for s, mi in enumerate((0, 3)):
                    kb.op("dve", lambda g, mi=mi, s=s: g.tensor_copy(SHT[:, l, v, s, :], MODS[:, l, v, mi, :]), [MODSb], [SHTb])
                for s, (mi, gt, gtb) in enumerate(((1, gmx, gmxb), (4, gfx, gfxb))):
                    kb.op("dve", lambda g, mi=mi, gt=gt, s=s: g.scalar_tensor_tensor(
                        AM[:, l, v, s, :], MODS[:, l, v, mi, :], 1.0, gt[:, l, :], ALU.add, ALU.mult),
                        [MODSb, gtb], [AMb])
        dg = [ph.sb("dg%d" % i, [128, 128], F32) for i in range(2)]
        gr = [ph.sb("gr%d" % i, [128, D], F32) for i in range(2)]
        n = 0
        for l in range(NLAYERS):
            for v in range(2):
                for s, mi in enumerate((2, 5)):
                    grt, grb = gr[(l * 4 + v * 2 + s) % 2]
                    for k4 in range(0, KC, 4):
                        pst, pbb = next_ps()
                        for j in range(4):
                            k = k4 + j
                            dt_, db_ = dg[n % 2]
                            n += 1
                            kb.op("dve", lambda g, k=k, dt_=dt_: g.tensor_scalar(dt_[:], ident[:], MODS[:, l, v, mi, k:k + 1], None, ALU.mult),
                                  [identb, MODSb], [db_])
                            kb.op("pe", lambda g, j=j, dt_=dt_: g.matmul(pst[:, j * 128:(j + 1) * 128], ones[:], dt_[:], start=True, stop=True),
                                  [onesb, db_], [pbb])
                        copy("act", grt[:, k4 * 128:(k4 + 4) * 128], pst[:], [pbb], [grb])
                    kb.dma("sp", [(GBC[l, v, s, :, :], grt[:])], [grb], [GBCb])

    dbg_out("modg", MODG, [NCORE * 128, L * 24 * 2], F32, [MODGb])
    dbg_out("gbc", GBC, [L, 2, 2, 128, D], F32, [GBCb])

    def wchunks(Wap, r0, kc_n, c0, ncols, wview):
        prs = []
        step = 8
        for k0 in range(0, kc_n, step):
            k1 = min(kc_n, k0 + step)
            prs.append((wview[:, k0:k1, :],
                        Wap[r0 + k0 * 128:r0 + k1 * 128, c0:c0 + ncols].rearrange("(k p) n -> p k n", p=128)))
        return prs

    WSLOT = KC * 256

    def rmsnorm_phase(l, s, router=None):
        msh = 0 if s == 0 else 3
        with Phase(kb) as ph:
            xts = [ph.sb("xt%d" % i, [128, D], F32) for i in range(2)]
            junk, junkb = ph.sb("junk", [128, D], F32)
            st = [ph.sb("st%d" % i, [128, 4], F32) for i in range(2)]
            h32 = [ph.sb("h32_%d" % i, [128, 128], F32) for i in range(3)] if router else None
            nh = 0
            for t in range(T):
                v = 0 if t < TL else 1
                xt, xb = xts[t % 2]
                stt, stb = st[t % 2]
                kb.dma("sp", [(xt[:], X[t * 128:(t + 1) * 128, :])], [Xb[t]], [xb])
                kb.op("dve", lambda g: g.memset(stt[:], 0.0), [], [stb])
                kb.op("act", lambda g: g.activation(junk[:], xt[:], AF.Square, accum_out=stt[:, 0:1]), [xb], [junkb, stb])
                kb.op("dve", lambda g: g.tensor_scalar(stt[:, 1:2], stt[:, 0:1], 1.0 / D, EPS, ALU.mult, ALU.add), [stb], [stb])
                kb.op("act", lambda g: g.activation(stt[:, 2:3], stt[:, 1:2], AF.Sqrt), [stb], [stb])
                kb.op("dve", lambda g: g.reciprocal(stt[:, 2:3], stt[:, 2:3]), [stb], [stb])
                kb.op("dve", lambda g: g.tensor_scalar(xt[:], xt[:], stt[:, 2:3], None, ALU.mult), [xb, stb], [xb])
                if router:
                    psr, psrb = psS, psSb
                for k4 in range(0, KC, 4):
                    pst, pbb = next_ps()
                    for j in range(4):
                        k = k4 + j
                        kb.op("pe", lambda g, j=j, k=k: g.transpose(pst[:, j * 128:(j + 1) * 128], xt[:, k * 128:(k + 1) * 128], ident[:]),
                              [xb, identb], [pbb])
                    for j in range(4):
                        k = k4 + j
                        dst = A[:, k, t * 128:(t + 1) * 128]
                        sc_ap = AM[:, l, v, s, k:k + 1]
                        bi_ap = SHT[:, l, v, s, k:k + 1]
                        if router:
                            ht, hb = h32[nh % 3]
                            nh += 1
                            kb.op("act", lambda g, j=j, ht=ht, sc_ap=sc_ap, bi_ap=bi_ap: g.activation(
                                ht[:], pst[:, j * 128:(j + 1) * 128], AF.Identity, bias=bi_ap, scale=sc_ap), [pbb, AMb, SHTb], [hb])
                            kb.op("dve", lambda g, ht=ht, dst=dst: g.tensor_copy(dst, ht[:]), [hb], [Ab])
                            kb.op("pe", lambda g, k=k, ht=ht: g.matmul(psr[:, 0:NE], ht[:], router[0][:, k, :], start=(k == 0), stop=(k == KC - 1)),
                                  [hb, router[1]], [psrb])
                        elif (j % 2) == 0:
                            kb.op("act", lambda g, j=j, dst=dst, sc_ap=sc_ap, bi_ap=bi_ap: g.activation(
                                dst, pst[:, j * 128:(j + 1) * 128], AF.Identity, bias=bi_ap, scale=sc_ap), [pbb, AMb, SHTb], [Ab])
                        else:
                            kb.op("dve", lambda g, j=j, dst=dst, sc_ap=sc_ap, bi_ap=bi_ap: g.tensor_scalar(
                                dst, pst[:, j * 128:(j + 1) * 128], sc_ap, bi_ap, ALU.mult, ALU.add), [pbb, AMb, SHTb], [Ab])
                if router:
                    router[2](t, psr, psrb)

    def gemm_tok(Wp, r0, kc_n, groups, act, actb, tiles, evac, ph, wsl, akoff=0):
        for gi, (c0, ncols) in enumerate(groups):
            (Wap, Wb), c0 = wres(Wp, c0)
            wt, wb = wsl[rr["w"] % 2]
            rr["w"] += 1
            wv = wt[:, 0:kc_n * ncols].rearrange("p (k n) -> p k n", n=ncols)
            kb.dma("pool", wchunks(Wap, r0, kc_n, c0, ncols, wv), [Wb], [wb])
            for t in tiles:
                pst, pbb = next_ps()
                for k in range(kc_n):
                    kb.op("pe", lambda g, k=k: g.matmul(pst[:, 0:ncols], act[:, akoff + k, t * 128:(t + 1) * 128], wv[:, k, :],
                                                        start=(k == 0), stop=(k == kc_n - 1)), [wb, actb], [pbb])
                evac(gi, t, pst, pbb)

    CHUNKS = [(0, 512), (512, 512), (1024, 256)]

    def wres(Wp, c0):
        if isinstance(Wp, list):
            for i, (cs, ce) in enumerate(WIN_SLABS):
                if cs <= c0 < ce:
                    return Wp[i], c0 - cs
            raise ValueError(c0)
        return Wp, c0

    def gemm_ft(Wp, r0, kc_n, groups, act, actb, chunks, evac, ph, wsl, akoff=0):
        for gi, (c0, ncols) in enumerate(groups):
            (Wap, Wb), c0 = wres(Wp, c0)
            wt, wb = wsl[rr["w"] % 2]
            rr["w"] += 1
            wv = wt[:, 0:kc_n * ncols].rearrange("p (k n) -> p k n", n=ncols)
            kb.dma("pool", wchunks(Wap, r0, kc_n, c0, ncols, wv), [Wb], [wb])
            for j in range(ncols // 128):
                for ci, (t0, tw) in enumerate(chunks):
                    pst, pbb = next_ps()
                    for k in range(kc_n):
                        kb.op("pe", lambda g, k=k: g.matmul(pst[:, 0:tw], wv[:, k, j * 128:(j + 1) * 128], act[:, akoff + k, t0:t0 + tw],
                                                            start=(k == 0), stop=(k == kc_n - 1)), [wb, actb], [pbb])
                    evac(gi, j, ci, pst, pbb)

    rr["w"] = 0

    def erow(t):
        return E_OWN + t * 128 if t < TL else E_CTX + (t - TL) * 128

    for l in range(NLAYERS):
        if l + 1 < NLAYERS:
            gather_layer(l + 1)
        li = l // 2
        moe = (l % 2 == 1)

        rmsnorm_phase(l, 0)

        with Phase(kb) as ph:
            wsl = [ph.sb("wsl%d" % i, [128, WSLOT], BF16) for i in range(2)]
            stg = [ph.sb("stg%d" % i, [128, 256], F32) for i in range(4)]
            stgh = [ph.sb("stgh%d" % i, [128, 512], BF16) for i in range(4)]
            cnt = [0]

            tokgroups = [(OFF_AU + i * 256, 256) for i in range(8)] + [(OFF_CQ + i * 256, 256) for i in range(4)]

            def evA(gi, t, pst, pbb):
                s_, sb_ = stg[cnt[0] % 4]
                cnt[0] += 1
                copy(ev_eng(), s_[:], pst[:, 0:256], [pbb], [sb_])
                kb.dma("sp", [(PXA[t * 128:(t + 1) * 128, gi * 256:(gi + 1) * 256], s_[:])], [sb_], [PXAb[t]])
            gemm_tok(W_in[l], 0, KC, tokgroups, A, Ab, range(T), evA, ph, wsl)

            kvgroups = [(OFF_BV + i * 256, 256) for i in range(6)]

            def evKV(gi, t, pst, pbb):
                s_, sb_ = stgh[cnt[0] % 4]
                cnt[0] += 1
                copy(ev_eng(), s_[:, 0:256], pst[:, 0:256], [pbb], [sb_])
                kb.dma("sp", [(EKV[erow(t):erow(t) + 128, gi * 256:(gi + 1) * 256], s_[:, 0:256])], [sb_], [EKVb])
            gemm_tok(W_in[l], 0, KC, kvgroups, A, Ab, range(T), evKV, ph, wsl)

            def ev_ft(dst, dstb, emap):
                def ev(gi, j, ci, pst, pbb):
                    t0, tw = CHUNKS[ci]
                    s_, sb_ = stgh[cnt[0] % 4]
                    cnt[0] += 1
                    copy(ev_eng(), s_[:, 0:tw], pst[:, 0:tw], [pbb], [sb_])
                    f0 = gi * 256 + j * 128
                    c_ = emap(t0)
                    kb.dma("sp", [(dst[f0:f0 + 128, c_:c_ + tw], s_[:, 0:tw])], [sb_], [dstb])
                return ev
            gemm_ft(W_in[l], 0, KC, [(OFF_BQ + i * 256, 256) for i in range(4)], A, Ab, CHUNKS,
                    ev_ft(QBT, QBTb, lambda t0: t0), ph, wsl)
            gemm_ft(W_in[l], 0, KC, [(OFF_BK + i * 256, 256) for i in range(4)], A, Ab, CHUNKS,
                    ev_ft(EKT, EKTb, lambda t0: (E_OWN + t0) if t0 < NLAT else E_CTX), ph, wsl)

        kb.dma("sp", [(PKT[0:256, :], EKV[E_OWN:E_OWN + 256, :]), (PKT[256:512, :], EKV[E_OWN + 768:E_OWN + 1024, :])], [EKVb], [PKTb])
        kb.dma("sp", [(PKF[:, 0:256], EKT[:, E_OWN:E_OWN + 256]), (PKF[:, 256:512], EKT[:, E_OWN + 768:E_OWN + 1024])], [EKTb], [PKFb])
        kb.allgather(PKT, GT, [PKTb], [GTb])
        kb.allgather(PKF, GF, [PKFb], [GFb])
        with Phase(kb) as ph:
            gts = [ph.sb("gts%d" % i, [128, NCORE, 1536], BF16) for i in range(2)]
            gfs = [ph.sb("gfs%d" % i, [128, NCORE, 512], BF16) for i in range(2)]
            so = [ph.sb("so%d" % i, [128, 512], BF16) for i in range(4)]
            sel32, sel32b = ph.sb("sel32", [128, 2, NCORE, 128], F32)
            SELh, SELhb = ph.sb("SELh", [128, 2, NCORE, 128], BF16)
            kb.dma("sp", [(sel32[:], sel_in[:, :, :, :])], [], [sel32b])
            kb.op("dve", lambda g: g.tensor_copy(SELh[:], sel32[:]), [sel32b], [SELhb])
            n = 0
            GTv = GT.ap().rearrange("(s r) c -> r s c", s=NCORE)
            for half in range(2):
                for tb in range(2):
                    g_, gb_ = gts[(half * 2 + tb) % 2]
                    r0 = (256 if half == 0 else 0) + tb * 128
                    kb.dma("sp", [(g_[:, s0:s0 + 2, :], GTv[r0:r0 + 128, s0:s0 + 2, :]) for s0 in range(0, NCORE, 2)], [GTb], [gb_])
                    for cb in range(3):
                        pst, pbb = next_ps()
                        for s_ in range(NCORE):
                            kb.op("pe", lambda g, s_=s_: g.matmul(pst[:], SELh[:, half, s_, :], g_[:, s_, cb * 512:(cb + 1) * 512],
                                                                  start=(s_ == 0), stop=(s_ == NCORE - 1)), [SELhb, gb_], [pbb])
                        o_, ob_ = so[n % 4]
                        n += 1
                        copy(ev_eng(), o_[:], pst[:], [pbb], [ob_])
                        er = (E_PREV if half == 0 else E_NEXT) + tb * 128
                        kb.dma("sp", [(EKV[er:er + 128, cb * 512:(cb + 1) * 512], o_[:])], [ob_], [EKVb])
            GFv = GF.ap().rearrange("(s f) c -> f s c", s=NCORE)
            for fb in range(8):
                g_, gb_ = gfs[fb % 2]
                kb.dma("sp", [(g_[:, s0:s0 + 4, :], GFv[fb * 128:(fb + 1) * 128, s0:s0 + 4, :]) for s0 in range(0, NCORE, 4)], [GFb], [gb_])
                for half in range(2):
                    c0 = 256 if half == 0 else 0
                    pst, pbb = next_ps()
                    for s_ in range(NCORE):
                        kb.op("pe", lambda g, s_=s_: g.matmul(pst[:, 0:256], SELh[:, half, s_, :], g_[:, s_, c0:c0 + 256],
                                                              start=(s_ == 0), stop=(s_ == NCORE - 1)), [SELhb, gb_], [pbb])
                    o_, ob_ = so[n % 4]
                    n += 1
                    copy(ev_eng(), o_[:, 0:256], pst[:, 0:256], [pbb], [ob_])
                    ec = E_PREV if half == 0 else E_NEXT
                    kb.dma("sp", [(EKT[fb * 128:(fb + 1) * 128, ec:ec + 256], o_[:, 0:256])], [ob_], [EKTb])

        if l == 0:
            dbg_out("pxa", PXA, [NT, 3072], F32, PXAb[:1])
            dbg_out("ekv", EKV, [E_TOT, 1536], BF16, [EKVb])
            dbg_out("ekt", EKT, [1024, E_TOT], BF16, [EKTb])
            dbg_out("qbt", QBT, [1024, NT], BF16, [QBTb])
        with Phase(kb) as phl:
            lng, lngb = phl.sb("lng", [128, 1024], F32)
            lnb, lnbb = phl.sb("lnb", [128, 1024], F32)
            wst32, wst32b = phl.sb("wst32", [128, 8, 128], F32)
            wst, wstb = phl.sb("wst", [128, 8, 128], BF16)
            bst, bstb = phl.sb("bst", [128, 8], F32)
            snk, snkb = phl.sb("snk", [128, 8], F32)
            ktx, ktxb = phl.sb("ktx", [128, 8, 256], BF16)
            vtx, vtxb = phl.sb("vtx", [128, 2, 1536], BF16)
            kcx, kcxb = phl.sb("kcx", [128, 2, 256], BF16)
            swm, swmb = phl.sb("swm", [128, 3, 384], F32)
            kb.dma("sp", [(lng[:], lng_in[l]), (lnb[:], lnb_in[l])], [], [lngb, lnbb])
            kb.dma("sp", [(wst32[:], wst_in[l]), (bst[:], bst_in[l]), (snk[:], sink_in[l])], [], [wst32b, bstb, snkb])
            kb.dma("sp", [(swm[:], swam_in.ap().rearrange("c p n -> p c n"))], [], [swmb])
            kb.op("dve", lambda g: g.tensor_copy(wst[:], wst32[:]), [wst32b], [wstb])
            kb.dma("sp", [(ktx[:], EKT.ap().rearrange("(h d) n -> d h n", d=128)[:, :, E_CTX:E_CTX + 256])], [EKTb], [ktxb])
            kb.dma("sp", [(vtx[:], EKV.ap().rearrange("(j p) c -> p j c", p=128)[:, 0:2, :])], [EKVb], [vtxb])
            for kv in range(2):
                for j in range(2):
                    ptt, ptbb = next_pt()
                    kb.op("pe", lambda g: g.transpose(ptt[:, 0:128], vtx[:, j, 1024 + kv * 128:1024 + (kv + 1) * 128], identh[:]),
                          [vtxb, identhb], [ptbb])
                    copy(ev_eng(), kcx[:, kv, j * 128:(j + 1) * 128], ptt[:, 0:128], [ptbb], [kcxb])

            def softmax_pv(ph, tagn, Wt, sc, scb, mxextra, vparts, h, yout, youtb, tmp):
                P, Pb, PT, PTb, sm, smb = tmp
                kb.op("dve", lambda g: g.memset(sm[:], 0.0), [], [smb])
                kb.op("dve", lambda g: g.reduce_max(sm[:, 0:1], sc[:, 0:Wt], AX.X), [scb], [smb])
                if mxextra is not None:
                    kb.op("dve", lambda g: g.tensor_tensor(sm[:, 0:1], sm[:, 0:1], mxextra, ALU.max), [smb, snkb], [smb])
                kb.op("dve", lambda g: g.tensor_scalar(sm[:, 1:2], sm[:, 0:1], -1.0, None, ALU.mult), [smb], [smb])
                kb.op("act", lambda g: g.activation(P[:, 0:Wt], sc[:, 0:Wt], AF.Exp, bias=sm[:, 1:2], scale=1.0, accum_out=sm[:, 2:3]),
                      [scb, smb], [Pb, smb])
                if mxextra is not None:
                    kb.op("act", lambda g: g.activation(sm[:, 3:4], mxextra, AF.Exp, bias=sm[:, 1:2], scale=1.0), [smb, snkb], [smb])
                    kb.op("dve", lambda g: g.tensor_tensor(sm[:, 2:3], sm[:, 2:3], sm[:, 3:4], ALU.add), [smb], [smb])
                kb.op("dve", lambda g: g.reciprocal(sm[:, 4:5], sm[:, 2:3]), [smb], [smb])
                ptt, ptbb = next_pt()
                chunks = []
                a = 0
                for (vt, vb_, vidx, nrow, coff) in vparts:
                    chunks.append((a, nrow, vt, vb_, vidx, coff))
                    a += nrow
                assert a == Wt, (a, Wt)
                for ci, (a, nrow, vt, vb_, vidx, coff) in enumerate(chunks):
                    kb.op("pe", lambda g, ci=ci, a=a, nrow=nrow: g.transpose(ptt[0:nrow, ci * 128:(ci + 1) * 128], P[:, a:a + nrow], identh[:]),
                          [Pb, identhb], [ptbb])
                nfull = 0
                while nfull < len(chunks) and chunks[nfull][1] == 128:
                    nfull += 1
                e1 = ev_eng()
                if nfull:
                    copy(e1, PT[:, 0:nfull * 128], ptt[:, 0:nfull * 128], [ptbb], [PTb])
                for ci in range(len(chunks)):
                    nrow = chunks[ci][1]
                    if nrow != 128 or ci >= nfull:
                        if ci < nfull:
                            continue
                        copy(e1, PT[0:nrow, ci * 128:(ci + 1) * 128], ptt[0:nrow, ci * 128:(ci + 1) * 128], [ptbb], [PTb])
                pso, psob = next_ps()
                for ci, (a, nrow, vt, vb_, vidx, coff) in enumerate(chunks):
                    kb.op("pe", lambda g, ci=ci, nrow=nrow, vt=vt, vidx=vidx, coff=coff: g.matmul(
                        pso[:, 0:128], PT[0:nrow, ci * 128:(ci + 1) * 128], vt[0:nrow, vidx, coff:coff + 128],
                        start=(ci == 0), stop=(ci == len(chunks) - 1)), [PTb, vb_], [psob])
                kb.op("dve", lambda g: g.tensor_scalar(yout[:, h * 128:(h + 1) * 128], pso[:, 0:128], sm[:, 4:5], None, ALU.mult),
                      [psob, smb], [youtb])

            def y_to_YT(ph, y, yb_, br, t, tagn):
                yT, yTb = ph
                ptt, ptbb = next_pt()
                for j in range(8):
                    kb.op("pe", lambda g, j=j: g.transpose(ptt[:, j * 128:(j + 1) * 128], y[:, j * 128:(j + 1) * 128], identh[:]),
                          [yb_, identhb], [ptbb])
                copy(ev_eng(), yT[:].rearrange("p j n -> p (j n)"), ptt[:, 0:1024], [ptbb], [yTb])
                kb.dma("sp", [(YT.ap().rearrange("(j p) n -> p j n", p=128)[:, br * 8:(br + 1) * 8, t * 128:(t + 1) * 128], yT[:])], [yTb], [YTb])

            for t in range(T):
                lat = t < TL
                with Phase(kb) as ph:
                    u, ub = ph.sb("u", [128, 1024], F32)
                    vv, vvb = ph.sb("v", [128, 1024], F32)
                    t1, t1b = ph.sb("t1", [128, 1024], F32)
                    t2, t2b = ph.sb("t2", [128, 1024], F32)
                    vn, vnb = ph.sb("vn", [128, 1024], BF16)
                    ya, yab = ph.sb("ya", [128, 1024], BF16)
                    yT_ = ph.sb("yT", [128, 8, 128], BF16)
                    stt, stb = ph.sb("gst", [128, 8], F32)
                    kb.dma("sp", [(u[:], PXA[t * 128:(t + 1) * 128, 0:1024]), (vv[:], PXA[t * 128:(t + 1) * 128, 1024:2048])],
                           [PXAb[t]], [ub, vvb])

                    def gelu(xt_, xb_):
                        kb.op("act", lambda g: g.activation(t1[:], xt_[:], AF.Square), [xb_], [t1b])
                        kb.op("dve", lambda g: g.tensor_scalar(t1[:], t1[:], 0.044715, 1.0, ALU.mult, ALU.add), [t1b], [t1b])
                        kb.op("dve", lambda g: g.tensor_tensor(t1[:], t1[:], xt_[:], ALU.mult), [t1b, xb_], [t1b])
                        kb.op("act", lambda g: g.activation(t2[:], t1[:], AF.Sigmoid, scale=1.5957691216057308), [t1b], [t2b])
                        kb.op("dve", lambda g: g.tensor_tensor(xt_[:], xt_[:], t2[:], ALU.mult), [t2b, xb_], [xb_])
                    gelu(u, ub)
                    gelu(vv, vvb)
                    kb.op("dve", lambda g: g.memset(stt[:], 0.0), [], [stb])
                    kb.op("dve", lambda g: g.reduce_sum(stt[:, 0:1], vv[:], AX.X), [vvb], [stb])
                    kb.op("dve", lambda g: g.tensor_scalar(stt[:, 1:2], stt[:, 0:1], -1.0 / 1024, None, ALU.mult), [stb], [stb])
                    kb.op("dve", lambda g: g.tensor_scalar(vv[:], vv[:], stt[:, 1:2], None, ALU.add), [vvb, stb], [vvb])
                    kb.op("act", lambda g: g.activation(t1[:], vv[:], AF.Square, accum_out=stt[:, 2:3]), [vvb], [t1b, stb])
                    kb.op("dve", lambda g: g.tensor_scalar(stt[:, 3:4], stt[:, 2:3], 1.0 / 1024, EPS, ALU.mult, ALU.add), [stb], [stb])
                    kb.op("act", lambda g: g.activation(stt[:, 4:5], stt[:, 3:4], AF.Sqrt), [stb], [stb])
                    kb.op("dve", lambda g: g.reciprocal(stt[:, 4:5], stt[:, 4:5]), [stb], [stb])
                    kb.op("dve", lambda g: g.scalar_tensor_tensor(t2[:], vv[:], stt[:, 4:5], lng[:], ALU.mult, ALU.mult), [vvb, stb, lngb], [t2b])
                    kb.op("dve", lambda g: g.tensor_tensor(vn[:], t2[:], lnb[:], ALU.add), [t2b, lnbb], [vnb])
                    for half in range(2):
                        pst, pbb = next_ps()
                        for gq in range(4):
                            g8 = half * 4 + gq
                            kb.op("pe", lambda g, gq=gq, g8=g8: g.matmul(pst[:, gq * 128:(gq + 1) * 128], wst[:, g8, :], vn[:, g8 * 128:(g8 + 1) * 128],
                                                                         start=True, stop=True), [wstb, vnb], [pbb])
                        for gq in range(4):
                            g8 = half * 4 + gq
                            kb.op("dve", lambda g, gq=gq, g8=g8: g.scalar_tensor_tensor(
                                ya[:, g8 * 128:(g8 + 1) * 128], pst[:, gq * 128:(gq + 1) * 128], bst[:, g8:g8 + 1], u[:, g8 * 128:(g8 + 1) * 128],
                                ALU.add, ALU.mult), [pbb, bstb, ub], [yab])
                    y_to_YT(yT_, ya, yab, 0, t, "a")

                with Phase(kb) as ph:
                    W = NA_W[t] if lat else 0
                    Wt = W + 256
                    qT, qTb = ph.sb("qT", [128, 8, 128], BF16)
                    vw, vwb = ph.sb("vw", [128, 6, 1024], BF16)
                    kts = [ph.sb("kt%d" % i, [128, NA_WMAX], BF16) for i in range(2)]
                    bis = [ph.sb("bi%d" % i, [128, NA_WMAX], F32) for i in range(2)]
                    scs = [ph.sb("sc%d" % i, [128, 1024], F32) for i in range(2)]
                    tmps = [(ph.sb("P%d" % i, [128, 1024], BF16) + ph.sb("PT%d" % i, [128, 1024], BF16) + ph.sb("sm%d" % i, [128, 8], F32))
                            for i in range(2)]
                    yb, ybb = ph.sb("yb", [128, 1024], BF16)
                    yT_ = ph.sb("yT", [128, 8, 128], BF16)
                    kb.dma("sp", [(qT[:], QBT.ap().rearrange("(h d) n -> d h n", d=128)[:, :, t * 128:(t + 1) * 128])], [QBTb], [qTb])
                    vparts_lat = []
                    if lat:
                        e0 = E_OWN + NA_START[t] * 64
                        nfull, rem = W // 128, W % 128
                        prs = [(vw[:, 0:nfull, :], EKV[e0:e0 + nfull * 128, 0:1024].rearrange("(j p) c -> p j c", p=128))]
                        if rem:
                            prs.append((vw[0:rem, nfull, :], EKV[e0 + nfull * 128:e0 + W, 0:1024]))
                        kb.dma("sp", prs, [EKVb], [vwb])
                        vparts_lat = [(vw, vwb, j, 128, 0) for j in range(nfull)] + ([(vw, vwb, nfull, rem, 0)] if rem else [])
                        cls = {0: 0, 1: 1, 6: 3, 7: 4}.get(t, 2)
                        boff = sum(NA_W[i] for i in NA_CLS[:cls])
                    for h in range(8):
                        sc, scb = scs[h % 2]
                        if lat:
                            kt, ktb = kts[h % 2]
                            bi, bib = bis[h % 2]
                            kb.dma("sp", [(kt[:, 0:W], EKT[h * 128:(h + 1) * 128, e0:e0 + W])], [EKTb], [ktb])
                            kb.dma("sp", [(bi[:, 0:W], nab_in[l, h, :, boff:boff + W])], [], [bib])
                            kb.op("pe", lambda g: g.matmul(psS[:, 0:512], qT[:, h, :], kt[:, 0:512], start=True, stop=True), [qTb, ktb], [psSb])
                            kb.op("pe", lambda g: g.matmul(psS[:, 512:W], qT[:, h, :], kt[:, 512:W], start=True, stop=True), [qTb, ktb], [psSb])
                        kb.op("pe", lambda g: g.matmul(psS[:, W:Wt], qT[:, h, :], ktx[:, h, :], start=True, stop=True), [qTb, ktxb], [psSb])
                        if lat:
                            kb.op("dve", lambda g: g.scalar_tensor_tensor(sc[:, 0:W], psS[:, 0:W], SCALE, bi[:, 0:W], ALU.mult, ALU.add),
                                  [psSb, bib], [scb])
                        kb.op("act", lambda g: g.activation(sc[:, W:Wt], psS[:, W:Wt], AF.Copy, scale=SCALE), [psSb], [scb])
                        vparts = vparts_lat + [(vtx, vtxb, j, 128, 0) for j in range(2)]
                        vparts = [(a_, b_, c_, d_, e_ + h * 128) for (a_, b_, c_, d_, e_) in vparts]
                        softmax_pv(ph, "b", Wt, sc, scb, None, vparts, h, yb, ybb, tmps[h % 2])
                    y_to_YT(yT_, yb, ybb, 1, t, "b")

                with Phase(kb) as ph:
                    W = 384 if lat else 0
                    Wt = W + 256
                    cq, cqb = ph.sb("cq", [128, 1024], F32)
                    r1, r1b = ph.sb("r1", [128, 1024], F32)
                    r2, r2b = ph.sb("r2", [128, 1024], F32)
                    rt, rtb = ph.sb("rt", [128, 2, 1024], F32)
                    qr, qrb = ph.sb("qr", [128, 1024], BF16)
                    qT, qTb = ph.sb("qTc", [128, 8, 128], BF16)
                    kw, kwb = ph.sb("kw", [128, 3, 512], BF16)
                    kr, krb = ph.sb("kr", [128, 3, 256], BF16)
                    kT, kTb = ph.sb("kT", [128, 2, 384], BF16)
                    scs = [ph.sb("scc%d" % i, [128, 1024], F32) for i in range(2)]
                    tmps = [(ph.sb("Pc%d" % i, [128, 1024], BF16) + ph.sb("PTc%d" % i, [128, 1024], BF16) + ph.sb("smc%d" % i, [128, 8], F32))
                            for i in range(2)]
                    yc, ycb = ph.sb("yc", [128, 1024], BF16)
                    yT_ = ph.sb("yT", [128, 8, 128], BF16)
                    kb.dma("sp", [(cq[:], PXA[t * 128:(t + 1) * 128, 2048:3072])], [PXAb[t]], [cqb])

                    def rope(src, srcb, dst, dstb, ncol, tab):
                        nh_ = ncol // 128
                        kb.op("dve", lambda g: g.tensor_tensor(r1[:, 0:ncol], src, tab[:, 0, 0:ncol], ALU.mult), [srcb, rtb], [r1b])
                        sv = src.rearrange("p (h a b c) -> p h a b c", h=nh_, a=2, b=2)
                        r2v = r2[:, 0:ncol].rearrange("p (h a b c) -> p h a b c", h=nh_, a=2, b=2)
                        tv = tab[:, 1, 0:ncol].rearrange("p (h a b c) -> p h a b c", h=nh_, a=2, b=2)
                        for a_ in range(2):
                            kb.op("dve", lambda g, a_=a_: g.tensor_tensor(r2v[:, :, a_, 0, :], sv[:, :, a_, 1, :], tv[:, :, a_, 0, :], ALU.mult), [srcb, rtb], [r2b])
                            kb.op("dve", lambda g, a_=a_: g.tensor_tensor(r2v[:, :, a_, 1, :], sv[:, :, a_, 0, :], tv[:, :, a_, 1, :], ALU.mult), [srcb, rtb], [r2b])
                        kb.op("dve", lambda g: g.tensor_tensor(dst, r1[:, 0:ncol], r2[:, 0:ncol], ALU.add), [r1b, r2b], [dstb])

                    if lat:
                        kb.dma("sp", [(rt[:], rope_in[128 + t * 128:128 + (t + 1) * 128, :, :])], [], [rtb])
                        rope(cq[:], cqb, qr[:], qrb, 1024, rt)
                    else:
                        kb.op("dve", lambda g: g.tensor_copy(qr[:], cq[:]), [cqb], [qrb])
                    ptt, ptbb = next_pt()
                    for j in range(8):
                        kb.op("pe", lambda g, j=j: g.transpose(ptt[:, j * 128:(j + 1) * 128], qr[:, j * 128:(j + 1) * 128], identh[:]),
                              [qrb, identhb], [ptbb])
                    copy(ev_eng(), qT[:].rearrange("p j n -> p (j n)"), ptt[:, 0:1024], [ptbb], [qTb])
                    if lat:
                        e0 = E_OWN + (t - 1) * 128
                        kb.dma("sp", [(kw[:], EKV[e0:e0 + 384, 1024:1536].rearrange("(j p) c -> p j c", p=128))], [EKVb], [kwb])
                        for j in range(3):
                            kb.dma("sp", [(rt[:], rope_in[(t + j) * 128:(t + j + 1) * 128, :, :])], [], [rtb])
                            rope(kw[:, j, 0:256], kwb, kr[:, j, :], krb, 256, rt)
                        ptt, ptbb = next_pt()
                        for kv in range(2):
                            for j in range(3):
                                kb.op("pe", lambda g, kv=kv, j=j: g.transpose(ptt[:, (kv * 3 + j) * 128:(kv * 3 + j + 1) * 128],
                                                                              kr[:, j, kv * 128:(kv + 1) * 128], identh[:]), [krb, identhb], [ptbb])
                        copy(ev_eng(), kT[:].rearrange("p a n -> p (a n)"), ptt[:, 0:768], [ptbb], [kTb])
                        cls = 0 if t == 0 else (2 if t == TL - 1 else 1)
                    for h in range(8):
                        kv = h // 4
                        sc, scb = scs[h % 2]
                        if lat:
                            kb.op("pe", lambda g: g.matmul(psS[:, 0:384], qT[:, h, :], kT[:, kv, :], start=True, stop=True), [qTb, kTb], [psSb])
                        kb.op("pe", lambda g: g.matmul(psS[:, 512:768], qT[:, h, :], kcx[:, kv, :], start=True, stop=True), [qTb, kcxb], [psSb])
                        if lat:
                            kb.op("dve", lambda g: g.scalar_tensor_tensor(sc[:, 0:W], psS[:, 0:W], SCALE, swm[:, cls, :], ALU.mult, ALU.add),
                                  [psSb, swmb], [scb])
                        kb.op("act", lambda g: g.activation(sc[:, W:Wt], psS[:, 512:768], AF.Copy, scale=SCALE), [psSb], [scb])
                        vparts = ([(kw, kwb, j, 128, 256 + kv * 128) for j in range(3)] if lat else []) + \
                                 [(vtx, vtxb, j, 128, 1280 + kv * 128) for j in range(2)]
                        softmax_pv(ph, "c", Wt, sc, scb, snk[:, h:h + 1], vparts, h, yc, ycb, tmps[h % 2])
                    y_to_YT(yT_, yc, ycb, 2, t, "c")

        if l == 0:
            dbg_out("yt", YT, [3072, NT], BF16, [YTb])
        with Phase(kb) as ph:
            wsl = [ph.sb("wsl%d" % i, [128, WSLOT], BF16) for i in range(2)]
            Bt, Bb = ph.sb("B", [128, 24, NT], BF16)
            gsig = [[ph.sb("gs%d_%d" % (j, ci), [128, 512], BF16) for ci in range(3)] for j in range(2)]
            acc = [[ph.sb("ac%d_%d" % (j, ci), [128, 512], F32) for ci in range(3)] for j in range(2)]
            tm = [ph.sb("tm%d" % i, [128, 512], F32) for i in range(2)]
            ao = [ph.sb("ao%d" % i, [128, 512], BF16) for i in range(2)]
            kb.dma("sp", [(Bt[:, j0:j0 + 4, :], YT.ap().rearrange("(j p) n -> p j n", p=128)[:, j0:j0 + 4, :]) for j0 in range(0, 24, 4)], [YTb], [Bb])
            n = [0]
            for gI in range(16):
                for i in range(3):
                    def ev_gate(gi, j, ci, pst, pbb):
                        tw = CHUNKS[ci][1]
                        gt_, gb_ = gsig[j][ci]
                        kb.op("act", lambda g: g.activation(gt_[:, 0:tw], pst[:, 0:tw], AF.Sigmoid), [pbb], [gb_])
                    gemm_ft(W_in[l], 0, KC, [(OFF_GATE + i * D + gI * 256, 256)], A, Ab, CHUNKS, ev_gate, ph, wsl)

                    def ev_br(gi, j, ci, pst, pbb, i=i, gI=gI):
                        t0, tw = CHUNKS[ci]
                        gt_, gb_ = gsig[j][ci]
                        at_, ab_ = acc[j][ci]
                        if i == 0:
                            kb.op("dve", lambda g: g.tensor_tensor(at_[:, 0:tw], pst[:, 0:tw], gt_[:, 0:tw], ALU.mult), [pbb, gb_], [ab_])
                        else:
                            tt_, tb_ = tm[n[0] % 2]
                            kb.op("dve", lambda g: g.tensor_tensor(tt_[:, 0:tw], pst[:, 0:tw], gt_[:, 0:tw], ALU.mult), [pbb, gb_], [tb_])
                            if i == 1:
                                kb.op("dve", lambda g: g.tensor_tensor(at_[:, 0:tw], at_[:, 0:tw], tt_[:, 0:tw], ALU.add), [tb_, ab_], [ab_])
                            else:
                                o_, ob_ = ao[n[0] % 2]
                                kb.op("dve", lambda g: g.tensor_tensor(o_[:, 0:tw], at_[:, 0:tw], tt_[:, 0:tw], ALU.add), [tb_, ab_], [ob_])
                                f0 = gI * 256 + j * 128
                                kb.dma("sp", [(ACCT[f0:f0 + 128, t0:t0 + tw], o_[:, 0:tw])], [ob_], [ACCTb])
                            n[0] += 1
                    gemm_ft(W_br[l], i * 1024, 8, [(gI * 256, 256)], Bt, Bb, CHUNKS, ev_br, ph, wsl, akoff=i * 8)

        def residual_gemm(Wp, kc_n, act, actb, s, ph, wsl):
            gb = [ph.sb("gb%d" % i, [128, 2, 256], F32) for i in range(2)]
            xs = [ph.sb("xs%d" % i, [128, 256], F32) for i in range(4)]
            tmr = [ph.sb("tmr%d" % i, [128, 256], F32) for i in range(2)]
            n = [0]

            def ev(gi, t, pst, pbb):
                v = 0 if t < TL else 1
                g_, gb_ = gb[gi % 2]
                if t == 0:
                    kb.dma("sp", [(g_[:, vv_, :], GBC[l, vv_, s, :, gi * 256:(gi + 1) * 256]) for vv_ in range(2)], [GBCb], [gb_])
                x_, xb_ = xs[n[0] % 4]
                t_, tb_ = tmr[n[0] % 2]
                n[0] += 1
                kb.dma("act", [(x_[:], X[t * 128:(t + 1) * 128, gi * 256:(gi + 1) * 256])], [Xb[t]], [xb_])
                kb.op("dve", lambda g: g.tensor_tensor(t_[:], pst[:, 0:256], g_[:, v, :], ALU.mult), [pbb, gb_], [tb_])
                kb.op("dve", lambda g: g.tensor_tensor(x_[:], x_[:], t_[:], ALU.add), [tb_, xb_], [xb_])
                kb.dma("sp", [(X[t * 128:(t + 1) * 128, gi * 256:(gi + 1) * 256], x_[:])], [xb_], [Xb[t]])
            gemm_tok(Wp, 0, kc_n, [(i * 256, 256) for i in range(16)], act, actb, range(T), ev, ph, wsl)

        with Phase(kb) as ph:
            wsl = [ph.sb("wsl%d" % i, [128, WSLOT], BF16) for i in range(2)]
            kb.dma("sp", [(A[:, k0:k0 + 4, :], ACCT.ap().rearrange("(k p) n -> p k n", p=128)[:, k0:k0 + 4, :]) for k0 in range(0, KC, 4)], [ACCTb], [Ab])
            residual_gemm(W_out[l], KC, A, Ab, 0, ph, wsl)

        if l == 0:
            dbg_out("acct", ACCT, [D, NT], BF16, [ACCTb])
            dbg_out("xmix", X, [NT, D], F32, Xb)
        if not moe:
            rmsnorm_phase(l, 1)
            with Phase(kb) as ph:
                wsl = [ph.sb("wsl%d" % i, [128, KC * 256], BF16) for i in range(2)]
                Bt, Bb = ph.sb("B", [128, 16, NT], BF16)
                sil = [[ph.sb("sil%d_%d" % (j, ci), [128, 512], F32) for ci in range(3)] for j in range(2)]
                for fg in range(8):
                    def ev_gate(gi, j, ci, pst, pbb):
                        tw = CHUNKS[ci][1]
                        s_, sb_ = sil[j][ci]
                        kb.op("act", lambda g: g.activation(s_[:, 0:tw], pst[:, 0:tw], AF.Silu), [pbb], [sb_])
                    gemm_ft(W_fg[li], 0, KC, [(fg * 256, 256)], A, Ab, CHUNKS, ev_gate, ph, wsl)

                    def ev_up(gi, j, ci, pst, pbb, fg=fg):
                        t0, tw = CHUNKS[ci]
                        s_, sb_ = sil[j][ci]
                        kb.op("dve", lambda g: g.tensor_tensor(Bt[:, fg * 2 + j, t0:t0 + tw], pst[:, 0:tw], s_[:, 0:tw], ALU.mult), [pbb, sb_], [Bb])
                    gemm_ft(W_fu[li], 0, KC, [(fg * 256, 256)], A, Ab, CHUNKS, ev_up, ph, wsl)
                residual_gemm(W_fd[li], 16, Bt, Bb, 1, ph, wsl)
        else:
            with Phase(kb) as phm:
                wr, wrb = phm.sb("wr", [128, KC, NE], F32)
                gate, gateb = phm.sb("gate", [128, T, NE], F32)
                kb.dma("sp", [(wr[:], wr_in[li])], [], [wrb])
                rst = [phm.sb("rst%d" % i, [128, 4, NE], F32) for i in range(2)]

                def router_done(t, psr, psrb):
                    r_, rb_ = rst[t % 2]
                    lg, m1, eq, l2 = r_[:, 0, :], r_[:, 1, 0:1], r_[:, 2, :], r_[:, 3, :]
                    m2, den = r_[:, 1, 1:2], r_[:, 1, 2:3]
                    kb.op("dve", lambda g: g.tensor_copy(lg, psr[:, 0:NE]), [psrb], [rb_])
                    kb.op("dve", lambda g: g.reduce_max(m1, lg, AX.X), [rb_], [rb_])
                    kb.op("dve", lambda g: g.tensor_scalar(eq, lg, m1, NEG, ALU.is_equal, ALU.mult), [rb_], [rb_])
                    kb.op("dve", lambda g: g.tensor_tensor(l2, lg, eq, ALU.add), [rb_], [rb_])
                    kb.op("dve", lambda g: g.reduce_max(m2, l2, AX.X), [rb_], [rb_])
                    kb.op("dve", lambda g: g.tensor_scalar(eq, lg, m2, None, ALU.is_ge), [rb_], [rb_])
                    kb.op("dve", lambda g: g.tensor_scalar(r_[:, 1, 3:4], m1, -1.0, None, ALU.mult), [rb_], [rb_])
                    kb.op("act", lambda g: g.activation(l2, lg, AF.Exp, bias=r_[:, 1, 3:4], scale=1.0), [rb_], [rb_])
                    kb.op("dve", lambda g: g.tensor_tensor(l2, l2, eq, ALU.mult), [rb_], [rb_])
                    kb.op("dve", lambda g: g.reduce_sum(den, l2, AX.X), [rb_], [rb_])
                    kb.op("dve", lambda g: g.reciprocal(den, den), [rb_], [rb_])
                    kb.op("dve", lambda g: g.tensor_scalar(gate[:, t, :], l2, den, None, ALU.mult), [rb_], [gateb])
                rmsnorm_phase(l, 1, router=(wr, wrb, router_done))
                with Phase(kb) as ph:
                    wsl = [ph.sb("wsl%d" % i, [128, KC * 192], BF16) for i in range(2)]
                    Bt, Bb = ph.sb("B", [128, 24, NT], BF16)
                    sil = [ph.sb("sil%d" % i, [128, T, 192], F32) for i in range(2)]
                    hidall, hidb = ph.sb("hid", [128, T, FE], BF16)
                    for e in range(NE):
                        for hf in range(2):
                            s_, sb_ = sil[hf]

                            def ev_gate(gi, t, pst, pbb, s_=s_, sb_=sb_):
                                kb.op("act", lambda g: g.activation(s_[:, t, :], pst[:, 0:192], AF.Silu), [pbb], [sb_])
                            gemm_tok(W_eg[li], e * D, KC, [(hf * 192, 192)], A, Ab, range(T), ev_gate, ph, wsl)
                        for hf in range(2):
                            s_, sb_ = sil[hf]

                            def ev_up(gi, t, pst, pbb, s_=s_, sb_=sb_, hf=hf, e=e):
                                h_, hb_ = hidall[:, t, :], hidb
                                kb.op("dve", lambda g: g.scalar_tensor_tensor(h_[:, hf * 192:(hf + 1) * 192], pst[:, 0:192], gate[:, t, e:e + 1], s_[:, t, :],
                                                                              ALU.mult, ALU.mult), [pbb, gateb, sb_], [hb_])
                                if hf == 1:
                                    ptt, ptbb = next_pt()
                                    for j in range(3):
                                        kb.op("pe", lambda g, j=j: g.transpose(ptt[:, j * 128:(j + 1) * 128], h_[:, j * 128:(j + 1) * 128], identh[:]),
                                              [hb_, identhb], [ptbb])
                                    for j in range(3):
                                        copy(ev_eng(), Bt[:, e * 3 + j, t * 128:(t + 1) * 128], ptt[:, j * 128:(j + 1) * 128], [ptbb], [Bb])
                            gemm_tok(W_eu[li], e * D, KC, [(hf * 192, 192)], A, Ab, range(T), ev_up, ph, wsl)
                    residual_gemm(W_ed[li], 24, Bt, Bb, 1, ph, wsl)

        wl = [b for (_, b) in W_in[l]] + [W_br[l][1], W_out[l][1]]
        wl += [W_eg[li][1], W_eu[li][1], W_ed[li][1]] if moe else [W_fg[li][1], W_fu[li][1], W_fd[li][1]]
        kb.retire(wl)
    dbg_out("xend", X, [NT, D], F32, Xb)
    with Phase(kb) as ph:
        gf, gfb = ph.sb("gf", [128, D], F32)
        xts = [ph.sb("xf%d" % i, [128, D], F32) for i in range(2)]
        junk, junkb = ph.sb("junk", [128, D], BF16)
        st = [ph.sb("stf%d" % i, [128, 4], F32) for i in range(2)]
        kb.dma("sp", [(gf[:], gfin_in[:, :])], [], [gfb])
        Yb = kb.buf("Y")
        for t in range(TL):
            xt, xb = xts[t % 2]
            stt, stb = st[t % 2]
            kb.dma("sp", [(xt[:], X[t * 128:(t + 1) * 128, :])], [Xb[t]], [xb])
            kb.op("dve", lambda g: g.memset(stt[:], 0.0), [], [stb])
            kb.op("act", lambda g: g.activation(junk[:], xt[:], AF.Square, accum_out=stt[:, 0:1]), [xb], [junkb, stb])
            kb.op("dve", lambda g: g.tensor_scalar(stt[:, 1:2], stt[:, 0:1], 1.0 / D, EPS, ALU.mult, ALU.add), [stb], [stb])
            kb.op("act", lambda g: g.activation(stt[:, 2:3], stt[:, 1:2], AF.Sqrt), [stb], [stb])
            kb.op("dve", lambda g: g.reciprocal(stt[:, 2:3], stt[:, 2:3]), [stb], [stb])
            kb.op("dve", lambda g: g.scalar_tensor_tensor(xt[:], xt[:], stt[:, 2:3], gf[:], ALU.mult, ALU.mult), [xb, stb, gfb], [xb])
            kb.dma("sp", [(y_out[t * 128:(t + 1) * 128, :], xt[:])], [xb], [Yb])
        kb.drain("sp", [Yb] + dbgb)
    return nc


def _na_bias(rpb_l, core):
    rows_total = 8192 // 64
    outs = []
    qc = np.arange(64)
    kc = np.arange(64)
    c0 = np.clip(qc - 8, 0, 48)
    in_win = (kc[None, :] >= c0[:, None]) & (kc[None, :] < c0[:, None] + 16)
    coff = np.clip(kc[None, :] - qc[:, None] + 15, 0, 30)
    for t in NA_CLS:
        nrows = NA_ROWS[t]
        W = nrows * 64
        b = np.full((8, 128, W), NEG, np.float32)
        for qr in range(2):
            r = core * 16 + 2 * t + qr
            ks = int(np.clip(r - 4, 0, rows_total - 8))
            for j in range(nrows):
                krow = core * 16 + NA_START[t] + j
                if krow < ks or krow >= ks + 8:
                    continue
                roff = krow - r + 7
                blk = rpb_l[:, roff, :][:, coff]
                blk = np.where(in_win[None], blk, np.float32(NEG))
                b[:, qr * 64:(qr + 1) * 64, j * 64:(j + 1) * 64] = blk
        outs.append(b)
    return np.concatenate(outs, axis=2)


def _host_inputs(inp):
    f32 = np.float32
    g = {k: np.asarray(v, dtype=f32) for k, v in inp.items()}
    x = g["x"][0]
    ctx = g["ctx"][0]
    cvec = np.stack([g["c"][0].reshape(KC, 128).T, g["c_ctx"].reshape(KC, 128).T], axis=-1)
    b_ada = g["b_ada"].reshape(L, NCORE, 24, 128).transpose(3, 1, 0, 2)
    g_mix = g["g_mix"].reshape(L, KC, 128).transpose(2, 0, 1)
    g_ffn = g["g_ffn"].reshape(L, KC, 128).transpose(2, 0, 1)
    ln_g = np.broadcast_to(g["gmlp_ln_g"][:, None, :], (L, 128, 1024))
    ln_b = np.broadcast_to(g["gmlp_ln_b"][:, None, :], (L, 128, 1024))
    ws_t = g["gmlp_ws"].transpose(0, 3, 1, 2)
    bs_t = g["gmlp_bs"].transpose(0, 2, 1)
    sink = np.broadcast_to(g["swa_sink"][:, None, :], (L, 128, 8))
    g_final = np.broadcast_to(g["g_final"][None, :], (128, D))
    w_router = g["w_router"].reshape(2, KC, 128, NE).transpose(0, 2, 1, 3)
    ident = np.eye(128, dtype=f32)
    inv = (10000.0 ** (-np.arange(0, 64, 2, dtype=f32) / 64)).astype(f32)
    a = np.arange(128)
    j = np.arange(384)
    rel = (j[None, :] - 128) - a[:, None]
    band = np.abs(rel) <= 128
    shared = dict(ctx=ctx, cvec=cvec, b_ada=b_ada, g_mix=g_mix, g_ffn=g_ffn, ln_g=ln_g, ln_b=ln_b, ws_t=ws_t, bs_t=bs_t,
                  sink=sink, g_final=g_final, w_router=w_router, ident_in=ident)
    shared = {k: np.ascontiguousarray(v, dtype=f32) for k, v in shared.items()}
    maps = []
    for c in range(NCORE):
        m = dict(shared)
        m["x"] = np.ascontiguousarray(x[c * NLAT:(c + 1) * NLAT])
        m["w_ada"] = np.ascontiguousarray(g["w_ada"][:NLAYERS, :, c * 3072:(c + 1) * 3072])
        m["na_bias"] = np.stack([_na_bias(g["na_rpb"][l], c) for l in range(L)])
        pos = np.arange(c * NLAT - 128, (c + 1) * NLAT + 128)
        ang_r = (pos // 64).astype(f32)[:, None] * inv[None, :]
        ang_c = (pos % 64).astype(f32)[:, None] * inv[None, :]
        C2 = np.concatenate([np.cos(ang_r), np.cos(ang_r), np.cos(ang_c), np.cos(ang_c)], axis=1)
        S2 = np.concatenate([-np.sin(ang_r), np.sin(ang_r), -np.sin(ang_c), np.sin(ang_c)], axis=1)
        m["rope"] = np.ascontiguousarray(np.stack([np.tile(C2, (1, 8)), np.tile(S2, (1, 8))], axis=1), dtype=f32)
        msk = np.zeros((3, 128, 384), f32)
        for cls in range(3):
            valid = band.copy()
            if cls == 0 and c == 0:
                valid[:, 0:128] = False
            if cls == 2 and c == NCORE - 1:
                valid[:, 256:384] = False
            msk[cls] = np.where(valid, 0.0, NEG)
        m["swa_mask"] = msk
        sel = np.zeros((128, 2, NCORE, 128), f32)
        if c > 0:
            sel[:, 0, c - 1, :] = ident
        if c < NCORE - 1:
            sel[:, 1, c + 1, :] = ident
        m["sel"] = sel
        for i, (cs, ce) in enumerate([(0, 4096), (4096, 8192), (8192, 12288), (12288, 16384), (16384, PTOT)]):
            m["w_in%d" % i] = np.ascontiguousarray(g["w_in"][:NLAYERS, c * 512:(c + 1) * 512, cs:ce])
        m["w_branch"] = np.ascontiguousarray(g["w_branch"].reshape(L, 3072, D)[:NLAYERS, c * 384:(c + 1) * 384, :])
        m["w_out"] = np.ascontiguousarray(g["w_out"][:NLAYERS, c * 512:(c + 1) * 512, :])
        m["w_ffn_gate"] = np.ascontiguousarray(g["w_ffn_gate"][:, c * 512:(c + 1) * 512, :])
        m["w_ffn_up"] = np.ascontiguousarray(g["w_ffn_up"][:, c * 512:(c + 1) * 512, :])
        m["w_ffn_down"] = np.ascontiguousarray(g["w_ffn_down"][:, c * 256:(c + 1) * 256, :])
        m["w_exp_gate"] = np.ascontiguousarray(g["w_exp_gate"][:, c])
        m["w_exp_up"] = np.ascontiguousarray(g["w_exp_up"][:, c])
        m["w_exp_down"] = np.ascontiguousarray(g["w_exp_down"][:, c])
        maps.append(m)
    return maps


_NC_CACHE = {}


def kernel(**inputs):
    maps = _host_inputs(inputs)
    if "nc" not in _NC_CACHE:
        _NC_CACHE["nc"] = build_program()
    res = run_bass_kernel_spmd(_NC_CACHE["nc"], maps, core_ids=list(range(NCORE)))
    if DEBUG:
        _NC_CACHE["res"] = res.results
    out = np.concatenate([res.results[c]["y"] for c in range(NCORE)], axis=0)
    return out.reshape(1, NCORE * NLAT, D).astype(np.float32)
```
